# Optimizing a Trainium2 kernel written in Bass

```python
import jax
import jax.numpy as jnp
from jax import lax
import numpy as np

D_MODEL = 1024
BATCH = 16
SEQ = 4096
DEPTH = 4

POOL_WINDOWS = (2, 4, 8, 16)
POOL_GROUP = D_MODEL // 8
D_POOL = POOL_GROUP * len(POOL_WINDOWS)
D_REC = D_MODEL // 2
N_REC_HEADS = 8
REC_HEAD = D_REC // N_REC_HEADS
CONV_WIDTH = 4
LRU_C = 8.0
D_IN_AB = D_POOL + 2 * D_REC
D_MIX_AB = D_POOL + D_REC
CHUNK = 128
D_SGU = D_MODEL
N_SGU_HEADS = 8
SGU_HEAD = D_SGU // N_SGU_HEADS
N_EXPERTS = 32
TOP_K = 4
D_FF = D_MODEL
SWIGLU_LIMIT = 7.0
SWIGLU_ALPHA = 1.702
EXPERT_BLOCK = 512
LN_EPS = 1e-5
DEEPNORM_ALPHA = (2 * DEPTH) ** 0.25
DEEPNORM_BETA = (8 * DEPTH) ** -0.25
N_EVEN = (DEPTH + 1) // 2
N_ODD = DEPTH // 2

kernel_name = "hybrid_pool_rglru_sgu_moe_trunk"


def _layer_norm(x, g, b):
    xf = x.astype(jnp.float32)
    mu = jnp.mean(xf, axis=-1, keepdims=True)
    var = jnp.mean(jnp.square(xf - mu), axis=-1, keepdims=True)
    return ((xf - mu) * lax.rsqrt(var + LN_EPS) * g + b).astype(x.dtype)


def _modulation(c_act, w_mod, b_mod):
    m = c_act @ w_mod + b_mod
    shift, scale, gate = jnp.split(m, 3, axis=-1)
    return shift[:, None], scale[:, None], 1.0 + gate[:, None]


def _pool_mixer(xp, pool_w, pool_scale):
    B_, S_, _ = xp.shape
    xf = xp.astype(jnp.float32)
    cs = jnp.pad(jnp.cumsum(xf, axis=1), ((0, 0), (1, 0), (0, 0)))
    t = jnp.arange(S_)
    outs = []
    for g, w in enumerate(POOL_WINDOWS):
        lo, hi = g * POOL_GROUP, (g + 1) * POOL_GROUP
        start = jnp.maximum(t + 1 - w, 0)
        window_sum = cs[:, 1:, lo:hi] - cs[:, start, lo:hi]
        count = jnp.minimum(t + 1, w).astype(jnp.float32)
        outs.append(window_sum / count[None, :, None] - xf[..., lo:hi])
    pooled = jnp.stack(outs, axis=2).astype(xp.dtype)
    mixed = jnp.einsum('bsgc,gcd->bsgd', pooled, pool_w)
    return mixed.reshape(B_, S_, D_POOL) * pool_scale


def _combine_linear(p, q):
    a1, b1 = p
    a2, b2 = q
    return a1 * a2, a2 * b1 + b2


def _rg_lru(xr, gr, conv_w, conv_b, w_a, b_a, w_x, b_x, lam):
    B_, S_, _ = xr.shape
    xpad = jnp.pad(xr, ((0, 0), (CONV_WIDTH - 1, 0), (0, 0)))
    xc = conv_b
    for k in range(CONV_WIDTH):
        xc = xc + xpad[:, k:k + S_] * conv_w[k]
    xh = xc.reshape(B_, S_, N_REC_HEADS, REC_HEAD)
    r = jax.nn.sigmoid(jnp.einsum('bshi,hij->bshj', xh, w_a).reshape(B_, S_, D_REC) + b_a)
    i = jax.nn.sigmoid(jnp.einsum('bshi,hij->bshj', xh, w_x).reshape(B_, S_, D_REC) + b_x)
    log_a = -LRU_C * r.astype(jnp.float32) * jax.nn.softplus(-lam.astype(jnp.float32))
    a = jnp.exp(log_a)
    mult = jnp.sqrt(-jnp.expm1(2.0 * log_a))
    b_in = mult * (i * xc).astype(jnp.float32)
    _, h = lax.associative_scan(_combine_linear, (a, b_in), axis=1)
    return h.astype(xr.dtype) * jax.nn.gelu(gr)


def _mixer_pool_lru(h, w_in, pool_w, pool_scale, conv_w, conv_b, w_a, b_a, w_x, b_x, lam,
                    w_out, b_out):
    z = h @ w_in
    xp = z[..., :D_POOL]
    xr = z[..., D_POOL:D_POOL + D_REC]
    gr = z[..., D_POOL + D_REC:]
    y_pool = _pool_mixer(xp, pool_w, pool_scale)
    y_rec = _rg_lru(xr, gr, conv_w, conv_b, w_a, b_a, w_x, b_x, lam)
    return jnp.concatenate([y_pool, y_rec], axis=-1) @ w_out + b_out


def _mixer_sgu(h, w_in, b_in, ln_g, ln_b, w_s, b_s, w_out, b_out):
    B_, S_, _ = h.shape
    z = jax.nn.gelu(h @ w_in + b_in)
    u, v = jnp.split(z, 2, axis=-1)
    v = _layer_norm(v, ln_g, ln_b)
    vc = v.reshape(B_, S_ // CHUNK, CHUNK, N_SGU_HEADS, SGU_HEAD)
    mask = jnp.tril(jnp.ones((CHUNK, CHUNK), dtype=bool))
    ws = jnp.where(mask[None], w_s, jnp.zeros_like(w_s))
    sv = jnp.einsum('hts,bnshc->bnthc', ws, vc) + b_s.T[None, None, :, :, None]
    return (u * sv.reshape(B_, S_, D_SGU)) @ w_out + b_out


def _moe_ffn(h, w_router, b_router, w1, b1, w2, b2):
    B_, S_, D = h.shape
    T = B_ * S_
    TK = T * TOP_K
    xt = h.reshape(T, D)
    logits = (xt @ w_router + b_router).astype(jnp.float32)
    top_vals, top_idx = lax.top_k(logits, TOP_K)
    gates = jax.nn.softmax(top_vals, axis=-1).astype(h.dtype)
    flat_e = top_idx.reshape(-1)
    flat_tok = jnp.repeat(jnp.arange(T, dtype=jnp.int32), TOP_K)
    flat_g = gates.reshape(-1)
    order = jnp.argsort(flat_e)
    se, stok, sg = flat_e[order], flat_tok[order], flat_g[order]
    counts = jnp.zeros((N_EXPERTS,), jnp.int32).at[flat_e].add(1)
    padded = ((counts + EXPERT_BLOCK - 1) // EXPERT_BLOCK) * EXPERT_BLOCK
    starts = jnp.cumsum(counts) - counts
    pends = jnp.cumsum(padded)
    pstarts = pends - padded
    dest = pstarts[se] + (jnp.arange(TK, dtype=jnp.int32) - starts[se])
    n_blocks = -(-TK // EXPERT_BLOCK) + N_EXPERTS
    cap = n_blocks * EXPERT_BLOCK
    buf_tok = jnp.full((cap,), T, jnp.int32).at[dest].set(stok)
    buf_g = jnp.zeros((cap,), h.dtype).at[dest].set(sg)
    block_start = jnp.arange(n_blocks, dtype=jnp.int32) * EXPERT_BLOCK
    block_e = jnp.clip(jnp.searchsorted(pends, block_start, side='right'), 0, N_EXPERTS - 1)
    x_pad = jnp.concatenate([xt, jnp.zeros((1, D), xt.dtype)], axis=0)

    def expert_block(args):
        tok_blk, g_blk, e = args
        hb = x_pad[tok_blk] @ w1[e] + b1[e]
        x_glu = jnp.minimum(hb[:, 0::2], SWIGLU_LIMIT)
        x_lin = jnp.clip(hb[:, 1::2], -SWIGLU_LIMIT, SWIGLU_LIMIT)
        act = x_glu * jax.nn.sigmoid(SWIGLU_ALPHA * x_glu) * (x_lin + 1.0)
        return (act @ w2[e] + b2[e]) * g_blk[:, None]

    yb = lax.map(expert_block, (buf_tok.reshape(n_blocks, EXPERT_BLOCK),
                                buf_g.reshape(n_blocks, EXPERT_BLOCK), block_e))
    out = jnp.zeros((T + 1, D), yb.dtype).at[buf_tok].add(yb.reshape(cap, D))[:T]
    return out.reshape(B_, S_, D)


def setup_inputs(seed: int = 0) -> dict:
    key = jax.random.key(seed)
    ks = iter(jax.random.split(key, 40))

    def nrm(shape, std):
        return jax.random.normal(next(ks), shape, jnp.float32) * std

    d = D_MODEL
    u = jax.random.uniform(next(ks), (N_EVEN, D_REC), jnp.float32, minval=0.9, maxval=0.999)
    a0 = u ** (1.0 / LRU_C)
    lru_lambda = jnp.log(a0) - jnp.log1p(-a0)
    return {
        'x': nrm((BATCH, SEQ, d), 1.0),
        'c': nrm((BATCH, d), 1.0),
        'mod_w': nrm((DEPTH, 2, d, 3 * d), 0.5 * d ** -0.5),
        'mod_b': nrm((DEPTH, 2, 3 * d), 0.01),
        'ln_g': 1.0 + nrm((DEPTH, 2, d), 0.05),
        'ln_b': nrm((DEPTH, 2, d), 0.01),
        'ab_w_in': nrm((N_EVEN, d, D_IN_AB), d ** -0.5),
        'pool_w': nrm((N_EVEN, len(POOL_WINDOWS), POOL_GROUP, POOL_GROUP), POOL_GROUP ** -0.5),
        'pool_scale': 1.0 + nrm((N_EVEN, D_POOL), 0.05),
        'conv_w': nrm((N_EVEN, CONV_WIDTH, D_REC), CONV_WIDTH ** -0.5),
        'conv_b': nrm((N_EVEN, D_REC), 0.01),
        'lru_w_a': nrm((N_EVEN, N_REC_HEADS, REC_HEAD, REC_HEAD), REC_HEAD ** -0.5),
        'lru_b_a': nrm((N_EVEN, D_REC), 0.01),
        'lru_w_x': nrm((N_EVEN, N_REC_HEADS, REC_HEAD, REC_HEAD), REC_HEAD ** -0.5),
        'lru_b_x': nrm((N_EVEN, D_REC), 0.01),
        'lru_lambda': lru_lambda,
        'ab_w_out': nrm((N_EVEN, D_MIX_AB, d), DEEPNORM_BETA * D_MIX_AB ** -0.5),
        'ab_b_out': nrm((N_EVEN, d), 0.01),
        'sgu_w_in': nrm((N_ODD, d, 2 * D_SGU), d ** -0.5),
        'sgu_b_in': nrm((N_ODD, 2 * D_SGU), 0.01),
        'sgu_ln_g': 1.0 + nrm((N_ODD, D_SGU), 0.05),
        'sgu_ln_b': nrm((N_ODD, D_SGU), 0.01),
        'sgu_w_s': nrm((N_ODD, N_SGU_HEADS, CHUNK, CHUNK), CHUNK ** -0.5),
        'sgu_b_s': 1.0 + nrm((N_ODD, N_SGU_HEADS, CHUNK), 0.01),
        'sgu_w_out': nrm((N_ODD, D_SGU, d), DEEPNORM_BETA * D_SGU ** -0.5),
        'sgu_b_out': nrm((N_ODD, d), 0.01),
        'router_w': nrm((DEPTH, d, N_EXPERTS), d ** -0.5),
        'router_b': nrm((DEPTH, N_EXPERTS), 0.01),
        'moe_w1': nrm((DEPTH, N_EXPERTS, d, 2 * D_FF), d ** -0.5),
        'moe_b1': nrm((DEPTH, N_EXPERTS, 2 * D_FF), 0.01),
        'moe_w2': nrm((DEPTH, N_EXPERTS, D_FF, d), DEEPNORM_BETA * D_FF ** -0.5),
        'moe_b2': nrm((DEPTH, N_EXPERTS, d), 0.01),
    }


def reference(x, c, mod_w, mod_b, ln_g, ln_b,
              ab_w_in, pool_w, pool_scale, conv_w, conv_b, lru_w_a, lru_b_a, lru_w_x, lru_b_x,
              lru_lambda, ab_w_out, ab_b_out,
              sgu_w_in, sgu_b_in, sgu_ln_g, sgu_ln_b, sgu_w_s, sgu_b_s, sgu_w_out, sgu_b_out,
              router_w, router_b, moe_w1, moe_b1, moe_w2, moe_b2):
    c_act = jax.nn.silu(c)
    for layer in range(DEPTH):
        j = layer // 2
        shift, scale, gate = _modulation(c_act, mod_w[layer, 0], mod_b[layer, 0])
        h = x * (1.0 + scale) + shift
        if layer % 2 == 0:
            y = _mixer_pool_lru(h, ab_w_in[j], pool_w[j], pool_scale[j], conv_w[j], conv_b[j],
                                lru_w_a[j], lru_b_a[j], lru_w_x[j], lru_b_x[j], lru_lambda[j],
                                ab_w_out[j], ab_b_out[j])
        else:
            y = _mixer_sgu(h, sgu_w_in[j], sgu_b_in[j], sgu_ln_g[j], sgu_ln_b[j], sgu_w_s[j],
                           sgu_b_s[j], sgu_w_out[j], sgu_b_out[j])
        x = _layer_norm(DEEPNORM_ALPHA * x + gate * y, ln_g[layer, 0], ln_b[layer, 0])
        shift, scale, gate = _modulation(c_act, mod_w[layer, 1], mod_b[layer, 1])
        h = x * (1.0 + scale) + shift
        y = _moe_ffn(h, router_w[layer], router_b[layer], moe_w1[layer], moe_b1[layer],
                     moe_w2[layer], moe_b2[layer])
        x = _layer_norm(DEEPNORM_ALPHA * x + gate * y, ln_g[layer, 1], ln_b[layer, 1])
    return x
```

```python
from contextlib import ExitStack
import math
import numpy as np
import concourse.bass as bass
import concourse.mybir as mybir
from concourse.bass_utils import run_bass_kernel_spmd

F32 = mybir.dt.float32
BF16 = mybir.dt.bfloat16
AF = mybir.ActivationFunctionType
ALU = mybir.AluOpType

import os
DEBUG_STOP = os.environ.get("DEBUG_STOP", "")
CAST_ENG = os.environ.get("CAST_ENG", "gpsimd")
NO_SAME = bool(os.environ.get("NO_SAME", ""))
SYNCW = bool(os.environ.get("SYNCW", ""))
NOCOMP = bool(os.environ.get("NOCOMP", ""))
NOLOAD = bool(os.environ.get("NOLOAD", ""))
NOMM2 = bool(os.environ.get("NOMM2", ""))
NOACT = bool(os.environ.get("NOACT", ""))
NOSTT = bool(os.environ.get("NOSTT", ""))
ONLY = os.environ.get("ONLY", "")

ENGS = ["sync", "scalar", "vector", "gpsimd", "tensor"]
COMPUTE = ["scalar", "vector", "gpsimd", "tensor"]

D = 1024
NE = 32
DEPTH = 4
ALPHA = (2 * DEPTH) ** 0.25
LN_EPS = 1e-5
SIGMAX = 1.0 / (1.0 + math.exp(-1.702 * 7.0))


class Prog:
    def __init__(self, nc, stack):
        self.nc = nc
        self.stack = stack
        self.ops = {e: [] for e in ENGS}
        self.sems = {}
        self.count = {}
        self.waited = {e: {} for e in ENGS}
        self.last_w = {}
        self.rd = {}
        self.segments = []
        self.cur_loop = None
        self.loopvar = 0
        for e in COMPUTE:
            self._mksem("p_" + e)

    def _mksem(self, key):
        if key not in self.sems:
            self.sems[key] = self.stack.enter_context(self.nc.semaphore(key))
            self.count[key] = 0
        return self.sems[key]

    def _deps(self, eng, reads, writes):
        deps = {}
        own = "p_" + eng
        for r in reads:
            t = self.last_w.get(r)
            if t is not None and deps.get(t[0], 0) < t[1]:
                deps[t[0]] = t[1]
            if r[0] == "B" and r[1:].isdigit():
                d = self.rd.get(r)
                if d:
                    for k, v in d.items():
                        if k != own and deps.get(k, 0) < v:
                            deps[k] = v
        for w in writes:
            t = self.last_w.get(w)
            if t is not None and deps.get(t[0], 0) < t[1]:
                deps[t[0]] = t[1]
            d = self.rd.get(w)
            if d:
                for k, v in d.items():
                    if deps.get(k, 0) < v:
                        deps[k] = v
        out = []
        wd = self.waited[eng]
        for k, v in deps.items():
            if eng == "tensor" and k == "p_tensor":
                continue
            if NO_SAME and k == "p_" + eng:
                continue
            if wd.get(k, 0) >= v:
                continue
            wd[k] = v
            out.append((k, v))
        return out

    def _mark(self, key, val, reads, writes):
        for w in writes:
            self.last_w[w] = (key, val)
            self.rd[w] = {}
        for r in reads:
            d = self.rd.setdefault(r, {})
            if d.get(key, 0) < val:
                d[key] = val

    def op(self, eng, fn, reads=(), writes=(), inc=True):
        if getattr(self, "skip_eng", None) == eng:
            return
        waits = self._deps(eng, reads, writes)
        key = "p_" + eng
        if inc:
            self.count[key] += 1
            val = self.count[key]
        else:
            val = self.count[key] + 1
        self.ops[eng].append((waits, fn, key if inc else None, 1))
        self._mark(key, val, reads, writes)

    def dma(self, q, stream, fn, reads=(), writes=()):
        key = "d_" + stream
        self._mksem(key)
        waits = self._deps(q, reads, writes)
        prev = self.count[key]
        if prev > 0 and self.waited[q].get(key, 0) < prev:
            self.waited[q][key] = prev
            waits.append((key, prev))
        self.count[key] += 16
        val = self.count[key]
        self.ops[q].append((waits, fn, key, 16))
        self._mark(key, val, reads, writes)

    def new_segment(self, loop=None):
        self._close_segment()
        self.ops = {e: [] for e in ENGS}
        for k in self.count:
            self.count[k] = 0
        self.waited = {e: {} for e in ENGS}
        self.last_w.clear()
        self.rd.clear()
        self.cur_loop = loop

    def barrier(self):
        self.new_segment(None)

    def _close_segment(self):
        if not any(self.ops[e] for e in ENGS):
            return
        waits = [(k, v) for k, v in self.count.items() if v > 0 and self.waited["sync"].get(k, 0) < v]
        if waits:
            self.ops["sync"].append((waits, None, None, 0))
        self.segments.append((self.cur_loop, self.ops))
        self.ops = {e: [] for e in ENGS}

    def _emit(self, ops):
        nc = self.nc
        prog = self
        with nc.Block() as b0:
            @b0.sync
            def _(sync):
                for sm in prog.sems.values():
                    sync.sem_clear(sm)
        with nc.Block() as block:
            def mk(ename):
                def body(eng):
                    for waits, fn, key, n in ops[ename]:
                        for k, v in waits:
                            eng.wait_ge(prog.sems[k], v)
                        if fn is None:
                            continue
                        ins = fn(eng)
                        if key is not None:
                            ins.then_inc(prog.sems[key], n)
                return body
            for ename in ENGS:
                if ops[ename]:
                    getattr(block, ename)(mk(ename))

    def _my_loop(self, start, end, step, ops):
        nc = self.nc
        ALL = mybir.ALL_ENGINES
        if getattr(self, "_loop_regs", None) is None:
            self._loop_regs = nc.alloc_registers("mk_loop_i", engines=ALL)
        regs = self._loop_regs
        lid = nc.next_id()
        l_start = "mk_loop_%d_loop" % lid
        l_end = "mk_loop_%d_end" % lid
        nc.regs_mov(regs, start)
        nc.br(l_start, engines=ALL)
        with nc.body(l_start, valid_engines=ALL):
            i = nc.snap(regs, min_val=start, max_val=end - step)
            self.loopvar = i
            self.loopoff = i
            self._emit(ops)
            nc.regs_alu(regs, regs, step, op=mybir.AluOpType.add)
            nc.br_lt(regs, end, on_true=l_start, on_false=l_end, engines=ALL)
        nc.switch_bb(l_end)

    def build(self):
        self._close_segment()
        for loop, ops in self.segments:
            if loop is not None:
                self._my_loop(loop[0] * 512, loop[1] * 512, 512, ops)
            else:
                self.loopvar = 0
                self._emit(ops)
        self.loopvar = 0


class StopBuild(Exception):
    pass


def dbg_stop(tag):
    if DEBUG_STOP == tag:
        raise StopBuild(tag)


class Arena:
    def __init__(self, t, ncols):
        self.t = t
        self.n = ncols
        self.off = 0

    def reset(self):
        self.off = 0

    def alloc(self, cols):
        a = self.off
        self.off += cols
        assert self.off <= self.n, (self.off, self.n)
        return self.t[:, a:a + cols]


A32_COLS = 21 * 1024
A16_COLS = 60 * 1024


def build_program(nb=2, S=4096, sublayers=tuple(range(8)), LW=4):
    nc = bass.Bass("TRN2", target_bir_lowering=False)
    T = nb * S
    dt = lambda name, shape, kind="ExternalInput": nc.dram_tensor(name, list(shape), F32, kind=kind).ap()
    x_in = dt("x", [T, D])
    cT_d = dt("cT", [128, 8, nb])
    LE = (LW + 1) // 2
    LO = max(LW // 2, 1)
    mod_w = dt("mod_w", [LW, 2, D, 3 * D]); mod_b = dt("mod_b", [LW, 2, 3 * D])
    ln_g = dt("ln_g", [LW, 2, D]); ln_b = dt("ln_b", [LW, 2, D])
    ab_w_in = dt("ab_w_in", [LE, D, 1536]); pool_w = dt("pool_w", [LE, 4, 128, 128])
    ab_vec = dt("ab_vec", [LE, 128, 40])
    lru_bd = dt("lru_bd", [LE, 2, 4, 128, 128])
    ab_w_out = dt("ab_w_out", [LE, D, D]); ab_b_out = dt("ab_b_out", [LE, D])
    sgu_w_in = dt("sgu_w_in", [LO, D, 2 * D]); sgu_buT = dt("sgu_buT", [LO, 128, 8])
    sgu_bv = dt("sgu_bv", [LO, D]); sgu_ln_g = dt("sgu_ln_g", [LO, D]); sgu_ln_b = dt("sgu_ln_b", [LO, D])
    sgu_wsT = dt("sgu_wsT", [LO, 8, 128, 128]); sgu_b_s = dt("sgu_b_s", [LO, 8 * 128])
    sgu_w_out = dt("sgu_w_out", [LO, D, D]); sgu_b_out = dt("sgu_b_out", [LO, D])
    router_w = dt("router_w", [LW, D, NE]); router_b = dt("router_b", [LW, NE])
    moe_w1 = dt("moe_w1", [LW, NE, D, 2 * D]); moe_b1T = dt("moe_b1T", [LW, 128, 8 * 2 * NE])
    moe_w2 = dt("moe_w2", [LW, NE, D, D]); moe_b2 = dt("moe_b2", [LW, NE, D])
    y = dt("y", [T, D], kind="ExternalOutput")
    m_dram = nc.dram_tensor("m_dram", [8, nb, 3 * D], F32).ap()

    with ExitStack() as st:
        P = Prog(nc, st)
        a32_t = st.enter_context(nc.sbuf_tensor("arena32", [128, A32_COLS], F32))
        a16_t = st.enter_context(nc.sbuf_tensor("arena16", [128, A16_COLS], BF16))
        ident = st.enter_context(nc.sbuf_tensor("ident", [128, 128], F32))
        ones = st.enter_context(nc.sbuf_tensor("ones", [128, 128], F32))
        psum = st.enter_context(nc.psum_tensor("psum", [128, 8, 512], F32))
        A32 = Arena(a32_t, A32_COLS)
        A16 = Arena(a16_t, A16_COLS)
        allow = st.enter_context(nc.allow_non_contiguous_dma(reason="small per-partition vector loads"))

        def bank(i):
            return psum[:, i, :]

        def drows(ap, base):
            i = P.loopvar
            if isinstance(i, int):
                return ap[base + 512 * i:base + 512 * i + 128, :]
            return ap[base:T, :][bass.ds(P.loopoff, 128), :]

        def dgroup(ap, base):
            i = P.loopvar
            if isinstance(i, int):
                v = ap[base + 512 * i:base + 512 * i + 512, :]
            else:
                v = ap[base:T, :][bass.ds(P.loopoff, 512), :]
            return v.rearrange("(t p) d -> p t d", p=128)

        def rows(base):
            i = P.loopvar
            if isinstance(i, int):
                return slice(base + 512 * i, base + 512 * i + 128)
            return bass.ds(i * 512 + base, 128)

        P.op("gpsimd", lambda e: e.memset(ones[:], 1.0), writes=["ones"])
        P.op("gpsimd", lambda e: e.memset(ident[:], 1.0), writes=["ident"])
        P.op("gpsimd", lambda e: e.affine_select(out=ident[:], in_=ident[:], pattern=[[1, 128]], base=0,
                                                 channel_multiplier=-1, compare_op=ALU.is_equal, fill=0.0),
             reads=["ident"], writes=["ident"])

        def emit_modulation():
            A32.reset()
            cT = A32.alloc(8 * nb).rearrange("p (k b) -> p k b", b=nb)
            cs = A32.alloc(8 * nb).rearrange("p (k b) -> p k b", b=nb)
            crep = A32.alloc(8 * nb * 128).rearrange("p (k b m) -> p k b m", b=nb, m=128)
            mw = [A32.alloc(8 * 512).rearrange("p (k n) -> p k n", n=512) for _ in range(2)]
            mb = [A32.alloc(512) for _ in range(2)]
            mres = [A32.alloc(512) for _ in range(2)]
            P.dma("sync", "m1", lambda e: e.dma_start(out=cT, in_=cT_d), writes=["cT"])
            P.op("scalar", lambda e: e.activation(out=cs, in_=cT, func=AF.Sigmoid), reads=["cT"], writes=["cs"])
            P.op("vector", lambda e: e.tensor_tensor(out=cs, in0=cs, in1=cT, op=ALU.mult), reads=["cs", "cT"], writes=["cs"])
            for k in range(8):
                for b in range(nb):
                    P.op("vector", lambda e, k=k, b=b: e.tensor_scalar(out=crep[:, k, b, :], in0=ones[:], scalar1=cs[:, k, b:b + 1],
                                                                     scalar2=None, op0=ALU.mult),
                         reads=["cs", "ones"], writes=["crep"])
            it = 0
            for sub in sorted(set(sublayers)):
                l, s = sub // 2, sub % 2
                for cb in range(6):
                    sl = it % 2
                    P.dma("sync", "mw%d" % sl, lambda e, l=l, s=s, cb=cb, sl=sl: e.dma_start(
                        out=mw[sl], in_=mod_w[l, s, :, cb * 512:(cb + 1) * 512].rearrange("(k p) n -> p k n", p=128)),
                        writes=["mw%d" % sl])
                    P.dma("sync", "mb%d" % sl, lambda e, l=l, s=s, cb=cb, sl=sl: e.dma_start(
                        out=mb[sl], in_=mod_b[l, s, cb * 512:(cb + 1) * 512].partition_broadcast(128)),
                        writes=["mb%d" % sl])
                    for b in range(nb):
                        pb = (it * nb + b) % 8
                        for k in range(8):
                            P.op("tensor", lambda e, k=k, b=b, sl=sl, pb=pb: e.matmul(bank(pb), lhsT=crep[:, k, b, :], rhs=mw[sl][:, k, :],
                                                                                    start=(k == 0), stop=(k == 7)),
                                 reads=["crep", "mw%d" % sl], writes=["B%d" % pb], inc=(k == 7))
                        rs = (it * nb + b) % 2
                        P.op("vector", lambda e, sl=sl, pb=pb, rs=rs: e.tensor_tensor(out=mres[rs], in0=bank(pb), in1=mb[sl], op=ALU.add),
                             reads=["B%d" % pb, "mb%d" % sl], writes=["mres%d" % rs])
                        if cb >= 2:
                            P.op("vector", lambda e, rs=rs: e.tensor_scalar(out=mres[rs], in0=mres[rs], scalar1=1.0, scalar2=None, op0=ALU.add),
                                 reads=["mres%d" % rs], writes=["mres%d" % rs])
                        P.dma("sync", "mst%d" % rs, lambda e, sub=sub, b=b, cb=cb, rs=rs: e.dma_start(
                            out=m_dram[sub, b:b + 1, cb * 512:(cb + 1) * 512], in_=mres[rs][0:1, :]),
                            reads=["mres%d" % rs], writes=["mdram"])
                    it += 1
            P.barrier()

        epsc = st.enter_context(nc.sbuf_tensor("epsc", [128, 1], F32))
        P.op("gpsimd", lambda e: e.memset(epsc[:], LN_EPS), writes=["epsc"])

        def load_mod_vectors(sub, b, shT, scT, gate_b, R):
            P.dma("sync", "m2", lambda e: e.dma_start(out=shT, in_=m_dram[sub, b, 0:D].rearrange("(k p) -> p k", p=128)),
                  writes=[R + "shT"])
            P.dma("sync", "m3", lambda e: e.dma_start(out=scT, in_=m_dram[sub, b, D:2 * D].rearrange("(k p) -> p k", p=128)),
                  writes=[R + "scT"])
            if gate_b is not None:
                P.dma("sync", "m4", lambda e: e.dma_start(out=gate_b, in_=m_dram[sub, b, 2 * D:3 * D].partition_broadcast(128)),
                      writes=[R + "gate"])

        def emit_moe(l, src):
            sub = 2 * l + 1
            A32.reset(); A16.reset()
            w1sb = [A16.alloc(8 * 2048).rearrange("p (k n) -> p k n", n=2048) for _ in range(2)]
            w2sb = [A16.alloc(8 * 1024).rearrange("p (k n) -> p k n", n=1024) for _ in range(2)]
            hT16 = A16.alloc(8 * 512).rearrange("p (k n) -> p k n", n=512)
            actT = [A16.alloc(8 * 512).rearrange("p (k n) -> p k n", n=512) for _ in range(2)]
            Rg = A32.alloc(6144)
            stg = [Rg[:, i * 2048:(i + 1) * 2048] for i in range(3)]
            gate_b = Rg[:, 0:1024]; lng_b = Rg[:, 1024:2048]; lnb_b = Rg[:, 2048:3072]
            ve = Rg[:, 4096:5120]
            xg3 = A32.alloc(4096).rearrange("p (t d) -> p t d", d=1024)
            hT32 = [Rg[:, 1024 + i * 2048:2048 + i * 2048].rearrange("p (k n) -> p k n", n=128) for i in range(2)]
            acc = [A32.alloc(1024) for _ in range(4)]
            wk = [[A32.alloc(512) for _ in range(3)] for _ in range(2)]
            b1sb = A32.alloc(512).rearrange("p (j t e) -> p j t e", t=2, e=NE)
            bgs = A32.alloc(256).rearrange("p (j e) -> p j e", e=NE)
            bl7 = A32.alloc(256).rearrange("p (j e) -> p j e", e=NE)
            b2sb = A32.alloc(1024)
            rw = A32.alloc(8 * NE).rearrange("p (k e) -> p k e", e=NE)
            rb_b = A32.alloc(NE)
            lg = A32.alloc(4 * NE).rearrange("p (t e) -> p t e", e=NE)
            ex = A32.alloc(NE); msk = A32.alloc(NE)
            G = A32.alloc(4 * NE).rearrange("p (t e) -> p t e", e=NE)
            GT = A32.alloc(512)
            top8 = A32.alloc(8); sm = A32.alloc(8)
            shT = A32.alloc(8); scT = A32.alloc(8)
            stt = A32.alloc(12); mvt = A32.alloc(8)

            P.dma("sync", "m5", lambda e: e.dma_start(out=b1sb.rearrange("p j t e -> p (j t e)"), in_=moe_b1T[l]), writes=["b1sb"])
            P.dma("sync", "m6", lambda e: e.dma_start(out=b2sb[0:NE, :], in_=moe_b2[l]), writes=["b2sb"])
            P.dma("sync", "m7", lambda e: e.dma_start(out=rw, in_=router_w[l].rearrange("(k p) e -> p k e", p=128)), writes=["rw"])
            P.dma("sync", "m8", lambda e: e.dma_start(out=rb_b, in_=router_b[l].partition_broadcast(128)), writes=["rb_b"])
            P.op("vector", lambda e: e.tensor_scalar(out=bgs, in0=b1sb[:, :, 0, :], scalar1=1.702, scalar2=None, op0=ALU.mult),
                 reads=["b1sb"], writes=["bgs"])
            P.op("vector", lambda e: e.tensor_scalar(out=bl7, in0=b1sb[:, :, 1, :], scalar1=7.0, scalar2=None, op0=ALU.add),
                 reads=["b1sb"], writes=["bl7"])

            pcs = [0]

            def wdma(e_, i):
                s_ = pcs[0] % 3
                pcs[0] += 1
                if i < 8:
                    P.dma("sync", "stg%d" % s_, lambda e: e.dma_start(out=stg[s_], in_=moe_w1[l, e_, i * 128:(i + 1) * 128, :]),
                          writes=["stg%d" % s_])
                else:
                    q = i - 8
                    P.dma("sync", "stg%d" % s_, lambda e: e.dma_start(
                        out=stg[s_].rearrange("p (k n) -> p k n", n=1024),
                        in_=moe_w2[l, e_, q * 256:(q + 1) * 256, :].rearrange("(k p) n -> p k n", p=128)),
                        writes=["stg%d" % s_])
                return s_

            def wcast(e_, i, s_):
                slot = e_ % 2
                if i < 8:
                    P.op("scalar", lambda e: e.activation(out=w1sb[slot][:, i, :].rearrange("p (t f) -> p f t", t=2),
                                                          in_=stg[s_].rearrange("p (f t) -> p f t", t=2), func=AF.Copy),
                         reads=["stg%d" % s_], writes=["w1_%d_%d" % (slot, i)])
                else:
                    q = i - 8
                    P.op("vector", lambda e: e.tensor_copy(out=w2sb[slot][:, 2 * q:2 * q + 2, :],
                                                           in_=stg[s_].rearrange("p (k n) -> p k n", n=1024)),
                         reads=["stg%d" % s_], writes=["w2_%d_%d" % (slot, q)])

            class WStream:
                def __init__(self, e_):
                    self.e_ = e_
                    self.slots = {}
                    self.nd = 0
                    self.ncast = 0
                    for _ in range(3):
                        self._dma()

                def _dma(self):
                    if self.nd < 12:
                        self.slots[self.nd] = wdma(self.e_, self.nd)
                        self.nd += 1

                def step(self):
                    if self.ncast < 12:
                        wcast(self.e_, self.ncast, self.slots[self.ncast])
                        self.ncast += 1
                        self._dma()

            def mm2(e_, slot, ab):
                if NOCOMP or NOMM2:
                    return
                for tt in range(4):
                    pb = 4 + 2 * (tt % 2)
                    for half in range(2):
                        for j in range(8):
                            P.op("tensor", lambda e, j=j, half=half, tt=tt, pb=pb: e.matmul(
                                bank(pb + half), lhsT=actT[ab][:, j, tt * 128:(tt + 1) * 128],
                                rhs=w2sb[slot][:, j, half * 512:(half + 1) * 512], start=(j == 0), stop=(j == 7)),
                                reads=["actT%d" % ab, "w2_%d_%d" % (slot, j // 2)], writes=["B%d" % (pb + half)], inc=(j == 7))
                    if NOSTT:
                        continue
                    P.op("vector", lambda e, tt=tt, pb=pb: e.scalar_tensor_tensor(
                        out=acc[tt], in0=psum[:, pb:pb + 2, :].rearrange("p a n -> p (a n)"), scalar=G[:, tt, e_:e_ + 1],
                        in1=acc[tt], op0=ALU.mult, op1=ALU.add),
                        reads=["B%d" % pb, "B%d" % (pb + 1), "G", "acc%d" % tt], writes=["acc%d" % tt])

            for b in range(nb):
                P.new_segment(loop=(0, S // 512))
                t0 = b * S
                load_mod_vectors(sub, b, shT, scT, None, "")
                P.dma("sync", "xg", lambda e, t0=t0: e.dma_start(out=xg3, in_=dgroup(src, t0)), reads=["xrows"], writes=["xg"])
                for tt in range(4):
                    xs = tt % 2
                    for half in range(2):
                        for k4 in range(4):
                            kc = half * 4 + k4
                            P.op("tensor", lambda e, half=half, k4=k4, kc=kc, tt=tt: e.transpose(
                                bank(half)[:, k4 * 128:(k4 + 1) * 128], xg3[:, tt, kc * 128:(kc + 1) * 128], ident[:]),
                                reads=["xg", "ident"], writes=["B%d" % half])
                        for k4 in range(4):
                            kc = half * 4 + k4
                            P.op("scalar", lambda e, half=half, k4=k4, kc=kc, xs=xs: e.activation(
                                out=hT32[xs][:, kc, :], in_=bank(half)[:, k4 * 128:(k4 + 1) * 128], func=AF.Identity,
                                bias=shT[:, kc:kc + 1], scale=scT[:, kc:kc + 1]),
                                reads=["B%d" % half, "shT", "scT"], writes=["stg%d" % xs])
                    P.op("vector", lambda e, tt=tt, xs=xs: e.tensor_copy(out=hT16[:, :, tt * 128:(tt + 1) * 128], in_=hT32[xs]),
                         reads=["stg%d" % xs], writes=["hT16"])
                    for k in range(8):
                        P.op("tensor", lambda e, k=k, tt=tt, xs=xs: e.matmul(bank(2)[:, tt * NE:(tt + 1) * NE], lhsT=hT32[xs][:, k, :],
                                                                         rhs=rw[:, k, :], start=(k == 0), stop=(k == 7)),
                             reads=["stg%d" % xs, "rw"], writes=["B2"], inc=(k == 7))
                    P.op("vector", lambda e, tt=tt: e.tensor_tensor(out=lg[:, tt, :], in0=bank(2)[:, tt * NE:(tt + 1) * NE], in1=rb_b, op=ALU.add),
                         reads=["B2", "rb_b"], writes=["lg"])
                    P.op("vector", lambda e, tt=tt: e.max(out=top8, in_=lg[:, tt, :]), reads=["lg"], writes=["top8"])
                    P.op("vector", lambda e: e.tensor_scalar(out=sm[:, 0:1], in0=top8[:, 0:1], scalar1=-1.0, scalar2=None, op0=ALU.mult),
                         reads=["top8"], writes=["sm0"])
                    P.op("scalar", lambda e, tt=tt: e.activation(out=ex, in_=lg[:, tt, :], func=AF.Exp, bias=sm[:, 0:1], scale=1.0),
                         reads=["lg", "sm0"], writes=["ex"])
                    P.op("vector", lambda e, tt=tt: e.tensor_scalar(out=msk, in0=lg[:, tt, :], scalar1=top8[:, 3:4], scalar2=None, op0=ALU.is_ge),
                         reads=["lg", "top8"], writes=["msk"])
                    P.op("vector", lambda e: e.tensor_tensor(out=ex, in0=ex, in1=msk, op=ALU.mult), reads=["ex", "msk"], writes=["ex"])
                    P.op("vector", lambda e: e.reduce_sum(out=sm[:, 1:2], in_=ex, axis=mybir.AxisListType.X), reads=["ex"], writes=["sm1"])
                    P.op("vector", lambda e: e.reciprocal(out=sm[:, 2:3], in_=sm[:, 1:2]), reads=["sm1"], writes=["sm2"])
                    P.op("vector", lambda e, tt=tt: e.tensor_scalar(out=G[:, tt, :], in0=ex, scalar1=sm[:, 2:3], scalar2=None, op0=ALU.mult),
                         reads=["ex", "sm2"], writes=["G"])
                    P.op("tensor", lambda e, tt=tt: e.transpose(bank(3)[0:NE, tt * 128:(tt + 1) * 128], G[:, tt, :], ident[:]),
                         reads=["G", "ident"], writes=["B3"])
                P.op("vector", lambda e: e.tensor_copy(out=GT[0:NE, :], in_=bank(3)[0:NE, :]), reads=["B3"], writes=["GT"])
                for tt in range(4):
                    pb = 4 + 2 * (tt % 2)
                    for half in range(2):
                        P.op("tensor", lambda e, tt=tt, half=half, pb=pb: e.matmul(
                            bank(pb + half), lhsT=GT[0:NE, tt * 128:(tt + 1) * 128], rhs=b2sb[0:NE, half * 512:(half + 1) * 512],
                            start=True, stop=True), reads=["GT", "b2sb"], writes=["B%d" % (pb + half)])
                    P.op("scalar", lambda e, tt=tt, pb=pb: e.activation(out=acc[tt], in_=psum[:, pb:pb + 2, :].rearrange("p a n -> p (a n)"),
                                                                      func=AF.Copy),
                         reads=["B%d" % pb, "B%d" % (pb + 1)], writes=["acc%d" % tt])
                dbg_stop("pro")
                ws0 = WStream(0)
                for i in range(12):
                    ws0.step()
                for e_ in range(NE):
                    slot = e_ % 2
                    ab = e_ % 2
                    nxt = (e_ + 1) % NE
                    last_inst = (e_ == NE - 1)
                    ws = None if (last_inst or NOLOAD) else WStream(nxt)
                    if SYNCW:
                        ws = None
                        if e_ > 0:
                            wsc = WStream(e_)
                            for _i in range(12):
                                wsc.step()
                    for j in range(8):
                        if ws is not None and j >= 1:
                            ws.step()
                        pp = j % 2
                        if NOCOMP:
                            continue
                        for half, boff in ((0, 0), (1, 1024)):
                            pbk = 2 * pp + half
                            for k in range(8):
                                P.op("tensor", lambda e, k=k, j=j, boff=boff, pbk=pbk, slot=slot: e.matmul(
                                    bank(pbk), lhsT=w1sb[slot][:, k, boff + j * 128:boff + (j + 1) * 128], rhs=hT16[:, k, :],
                                    start=(k == 0), stop=(k == 7)),
                                    reads=["w1_%d_%d" % (slot, k), "hT16"], writes=["B%d" % pbk], inc=(k == 7))
                        dbg_stop("mm1_%d_%d" % (e_, j))
                        if NOACT:
                            continue
                        sg, g1, rr = wk[pp]
                        W = "wk%d" % pp
                        P.skip_eng = {"act": "vector", "dve": "scalar"}.get(ONLY)
                        P.op("vector", lambda e, j=j, pp=pp, g1=g1, e_=e_: e.tensor_scalar(out=g1, in0=bank(2 * pp), scalar1=b1sb[:, j, 0, e_:e_ + 1],
                                                                                    scalar2=7.0, op0=ALU.add, op1=ALU.min),
                             reads=["B%d" % (2 * pp), "b1sb"], writes=[W + "g1"])
                        P.op("scalar", lambda e, j=j, pp=pp, rr=rr, e_=e_: e.activation(out=rr, in_=bank(2 * pp + 1), func=AF.Relu,
                                                                                 bias=bl7[:, j, e_:e_ + 1], scale=1.0),
                             reads=["B%d" % (2 * pp + 1), "bl7"], writes=[W + "r"])
                        P.op("scalar", lambda e, sg=sg, g1=g1: e.activation(out=sg, in_=g1, func=AF.Sigmoid, scale=1.702),
                             reads=[W + "g1"], writes=[W + "sg"])
                        P.op("vector", lambda e, rr=rr: e.tensor_scalar(out=rr, in0=rr, scalar1=-6.0, scalar2=8.0, op0=ALU.add, op1=ALU.min),
                             reads=[W + "r"], writes=[W + "r"])
                        P.op("vector", lambda e, sg=sg, g1=g1: e.tensor_tensor(out=g1, in0=sg, in1=g1, op=ALU.mult),
                             reads=[W + "sg", W + "g1"], writes=[W + "g1"])
                        P.op("vector", lambda e, j=j, ab=ab, g1=g1, rr=rr: e.tensor_tensor(out=actT[ab][:, j, :], in0=g1, in1=rr, op=ALU.mult),
                             reads=[W + "g1", W + "r"], writes=["actT%d" % ab])
                        P.skip_eng = None
                        dbg_stop("act_%d_%d" % (e_, j))
                    dbg_stop("j%d" % e_)
                    if ws is not None:
                        ws.step()
                    if e_ > 0:
                        mm2(e_ - 1, (e_ - 1) % 2, (e_ - 1) % 2)
                    dbg_stop("e%d" % e_)
                    if ws is not None:
                        for q in range(4):
                            ws.step()
                mm2(NE - 1, (NE - 1) % 2, (NE - 1) % 2)
                dbg_stop("exp")
                P.dma("sync", "m9", lambda e, b=b: e.dma_start(out=gate_b, in_=m_dram[sub, b, 2 * D:3 * D].partition_broadcast(128)),
                      writes=["stg0"])
                P.dma("sync", "m10", lambda e: e.dma_start(out=lng_b, in_=ln_g[l, 1].partition_broadcast(128)), writes=["stg0"])
                P.dma("sync", "m11", lambda e: e.dma_start(out=lnb_b, in_=ln_b[l, 1].partition_broadcast(128)), writes=["stg1"])
                dbg_stop("ep0")
                for tt in range(4):
                    emit_ln_epilogue_moe(xg3[:, tt, :], acc[tt], "acc%d" % tt, ve, gate_b, lng_b, lnb_b, stt, mvt)
                group_store(xg3, t0)
            P.new_segment()

        def emit_ln_epilogue_moe(xe, ytile, yres, ve, gate_b, lng_b, lnb_b, stt, mvt):
            P.op("vector", lambda e: e.tensor_tensor(out=ve, in0=ytile, in1=gate_b, op=ALU.mult), reads=[yres, "stg0"], writes=["stg2"])
            P.op("vector", lambda e: e.scalar_tensor_tensor(out=ve, in0=xe, scalar=ALPHA, in1=ve, op0=ALU.mult, op1=ALU.add),
                 reads=["xg", "stg2"], writes=["stg2"])
            ln_tail(xe, ve, "stg2", lng_b, "stg0", lnb_b, "stg1", stt, mvt)

        def ln_tail(outap, ve, vres, lng_b, gres, lnb_b, bres, stt, mvt):
            for h in range(2):
                P.op("vector", lambda e, h=h: e.bn_stats(out=stt[:, h * 6:(h + 1) * 6], in_=ve[:, h * 512:(h + 1) * 512]),
                     reads=[vres], writes=["stt"])
            P.op("vector", lambda e: e.bn_aggr(out=mvt[:, 0:2], in_=stt[:, 0:12]), reads=["stt"], writes=["mv"])
            dbg_stop("ep2")
            P.op("scalar", lambda e: e.activation(out=mvt[:, 2:3], in_=mvt[:, 1:2], func=AF.Sqrt, bias=epsc[:, 0:1], scale=1.0),
                 reads=["mv", "epsc"], writes=["sd"])
            P.op("vector", lambda e: e.reciprocal(out=mvt[:, 3:4], in_=mvt[:, 2:3]), reads=["sd"], writes=["rstd"])
            P.op("vector", lambda e: e.tensor_scalar(out=mvt[:, 4:5], in0=mvt[:, 0:1], scalar1=mvt[:, 3:4], scalar2=-1.0,
                                                     op0=ALU.mult, op1=ALU.mult), reads=["mv", "rstd"], writes=["nmr"])
            P.op("scalar", lambda e: e.activation(out=ve, in_=ve, func=AF.Identity, bias=mvt[:, 4:5], scale=mvt[:, 3:4]),
                 reads=[vres, "nmr", "rstd"], writes=[vres])
            P.op("vector", lambda e: e.tensor_tensor(out=ve, in0=ve, in1=lng_b, op=ALU.mult), reads=[vres, gres], writes=[vres])
            P.op("vector", lambda e: e.tensor_tensor(out=outap, in0=ve, in1=lnb_b, op=ALU.add), reads=[vres, bres, "xg"], writes=["xg"])


        onec = st.enter_context(nc.sbuf_tensor("onec", [128, 1], F32))
        zeroc = st.enter_context(nc.sbuf_tensor("zeroc", [128, 1], F32))
        P.op("gpsimd", lambda e: e.memset(onec[:], 1.0), writes=["onec"])
        P.op("gpsimd", lambda e: e.memset(zeroc[:], 0.0), writes=["zeroc"])

        def bload(dst, src_ap, name):
            P.dma("sync", "bl_" + name, lambda e: e.dma_start(out=dst, in_=src_ap.partition_broadcast(128)), writes=[name])

        def mixer_prologue(src, t0, hT16, xg3, shT, scT):
            P.dma("sync", "xg", lambda e: e.dma_start(out=xg3, in_=dgroup(src, t0)), reads=["xrows"], writes=["xg"])
            for tt in range(4):
                for half in range(2):
                    for k4 in range(4):
                        kc = half * 4 + k4
                        P.op("tensor", lambda e, half=half, k4=k4, kc=kc, tt=tt: e.transpose(
                            bank(half)[:, k4 * 128:(k4 + 1) * 128], xg3[:, tt, kc * 128:(kc + 1) * 128], ident[:]),
                            reads=["xg", "ident"], writes=["B%d" % half])
                    for k4 in range(4):
                        kc = half * 4 + k4
                        P.op("scalar", lambda e, half=half, k4=k4, kc=kc, tt=tt: e.activation(
                            out=hT16[:, kc, tt * 128:(tt + 1) * 128], in_=bank(half)[:, k4 * 128:(k4 + 1) * 128], func=AF.Identity,
                            bias=shT[:, kc:kc + 1], scale=scT[:, kc:kc + 1]),
                            reads=["B%d" % half, "shT", "scT"], writes=["hT16"])

        def group_store(xg3, t0):
            P.dma("sync", "xst", lambda e: e.dma_start(out=dgroup(y, t0), in_=xg3), reads=["xg"], writes=["xrows"])

        def ln_epilogue(xe, ytile, yres, bias_b, ve, gate_b, lng_b, lnb_b, stt, mvt):
            P.op("vector", lambda e: e.tensor_tensor(out=ve, in0=ytile, in1=bias_b, op=ALU.add), reads=list(yres) + ["bout_b"], writes=["ve"])
            P.op("vector", lambda e: e.tensor_tensor(out=ve, in0=ve, in1=gate_b, op=ALU.mult), reads=["ve", "gate_b"], writes=["ve"])
            P.op("vector", lambda e: e.scalar_tensor_tensor(out=ve, in0=xe, scalar=ALPHA, in1=ve, op0=ALU.mult, op1=ALU.add),
                 reads=["xg", "ve"], writes=["ve"])
            ln_tail(xe, ve, "ve", lng_b, "lng_b", lnb_b, "lnb_b", stt, mvt)

        def load_weights_bf16(pieces):
            for i, (src_ap, dst, view, res, eng) in enumerate(pieces):
                s_ = i % 3
                sv = view(stg_sl[s_])
                P.dma("sync", "stg%d" % s_, lambda e, sv=sv, src_ap=src_ap: e.dma_start(out=sv, in_=src_ap), writes=["stg%d" % s_])
                if eng == "scalar":
                    P.op("scalar", lambda e, sv=sv, dst=dst: e.activation(out=dst, in_=sv, func=AF.Copy), reads=["stg%d" % s_], writes=[res])
                else:
                    P.op("vector", lambda e, sv=sv, dst=dst: e.tensor_copy(out=dst, in_=sv), reads=["stg%d" % s_], writes=[res])

        stg_sl = [None, None, None]

        def emit_sgu(l, src):
            sub = 2 * l
            j = l // 2
            A32.reset(); A16.reset()
            winsb = A16.alloc(8 * 2048).rearrange("p (k n) -> p k n", n=2048)
            woutsb = A16.alloc(8 * 1024).rearrange("p (k n) -> p k n", n=1024)
            wssb = A16.alloc(8 * 128).rearrange("p (h t) -> p h t", t=128)
            hT16 = A16.alloc(8 * 512).rearrange("p (k n) -> p k n", n=512)
            uT = A16.alloc(8 * 512).rearrange("p (k n) -> p k n", n=512)
            gT16 = A16.alloc(8 * 512).rearrange("p (k n) -> p k n", n=512)
            vln16 = [A16.alloc(1024) for _ in range(4)]
            stgR = A32.alloc(7168)
            for i in range(3):
                stg_sl[i] = stgR[:, i * 2048:(i + 1) * 2048]
            xg3 = stgR[:, 0:4096].rearrange("p (t d) -> p t d", d=1024)
            ve = stgR[:, 4096:5120]
            vtm = [stgR[:, 5120:6144], stgR[:, 6144:7168]]
            bv_b = A32.alloc(1024); lnvg_b = A32.alloc(1024); lnvb_b = A32.alloc(1024); bs_b = A32.alloc(1024)
            bout_b = A32.alloc(1024); gate_b = A32.alloc(1024); lng_b = A32.alloc(1024); lnb_b = A32.alloc(1024)
            svt = [A32.alloc(512) for _ in range(2)]
            buT = A32.alloc(8); shT = A32.alloc(8); scT = A32.alloc(8); stt = A32.alloc(12); mvt = A32.alloc(8)
            stt2 = A32.alloc(12); mvt2 = A32.alloc(8)

            pieces = []
            for kc in range(8):
                pieces.append((sgu_w_in[j, kc * 128:(kc + 1) * 128, :], winsb[:, kc, :], (lambda a: a), "winsb", "scalar"))
            for q in range(4):
                pieces.append((sgu_w_out[j, q * 256:(q + 1) * 256, :].rearrange("(k p) n -> p k n", p=128), woutsb[:, 2 * q:2 * q + 2, :],
                               (lambda a: a.rearrange("p (k n) -> p k n", n=1024)), "woutsb", "vector"))
            load_weights_bf16(pieces)
            wsv = stg_sl[0][:, 0:1024].rearrange("p (h t) -> p h t", t=128)
            P.dma("sync", "stg0", lambda e: e.dma_start(out=wsv, in_=sgu_wsT[j].rearrange("h s t -> s h t")), writes=["stg0"])
            P.op("gpsimd", lambda e: e.affine_select(out=wsv, in_=wsv, pattern=[[0, 8], [1, 128]], base=0, channel_multiplier=-1,
                                                     compare_op=ALU.is_ge, fill=0.0), reads=["stg0"], writes=["stg0"])
            P.op("vector", lambda e: e.tensor_copy(out=wssb, in_=wsv), reads=["stg0"], writes=["wssb"])
            P.dma("sync", "buT", lambda e: e.dma_start(out=buT, in_=sgu_buT[j]), writes=["buT"])
            bload(bv_b, sgu_bv[j], "bv_b"); bload(lnvg_b, sgu_ln_g[j], "lnvg_b"); bload(lnvb_b, sgu_ln_b[j], "lnvb_b")
            bload(bs_b, sgu_b_s[j], "bs_b"); bload(bout_b, sgu_b_out[j], "bout_b")
            bload(lng_b, ln_g[l, 0], "lng_b"); bload(lnb_b, ln_b[l, 0], "lnb_b")
            P.barrier()

            for g in range(T // 512):
                t0 = g * 512
                b = t0 // S
                if t0 % S == 0:
                    P.new_segment(None)
                    bload(gate_b, m_dram[sub, b, 2 * D:3 * D], "gate_b")
                    load_mod_vectors(sub, b, shT, scT, None, "")
                mixer_prologue(src, t0, hT16, xg3, shT, scT)
                for c in range(8):
                    pb = 2 + c % 2
                    for k in range(8):
                        P.op("tensor", lambda e, k=k, c=c, pb=pb: e.matmul(bank(pb), lhsT=winsb[:, k, c * 128:(c + 1) * 128], rhs=hT16[:, k, :],
                                                                      start=(k == 0), stop=(k == 7)),
                             reads=["winsb", "hT16"], writes=["B%d" % pb], inc=(k == 7))
                    P.op("scalar", lambda e, c=c, pb=pb: e.activation(out=uT[:, c, :], in_=bank(pb), func=AF.Gelu_apprx_tanh,
                                                                    bias=buT[:, c:c + 1], scale=1.0),
                         reads=["B%d" % pb, "buT"], writes=["uT"])
                for tt in range(4):
                    vt = vtm[tt % 2]
                    V = "vtm%d" % (tt % 2)
                    for half in range(2):
                        for k in range(8):
                            P.op("tensor", lambda e, k=k, tt=tt, half=half: e.matmul(
                                bank(4 + half), lhsT=hT16[:, k, tt * 128:(tt + 1) * 128], rhs=winsb[:, k, D + half * 512:D + (half + 1) * 512],
                                start=(k == 0), stop=(k == 7)), reads=["winsb", "hT16"], writes=["B%d" % (4 + half)], inc=(k == 7))
                    P.op("vector", lambda e, vt=vt: e.tensor_tensor(out=vt, in0=psum[:, 4:6, :].rearrange("p a n -> p (a n)"), in1=bv_b, op=ALU.add),
                         reads=["B4", "B5", "bv_b"], writes=[V])
                    P.op("scalar", lambda e, vt=vt: e.activation(out=vt, in_=vt, func=AF.Gelu_apprx_tanh), reads=[V], writes=[V])
                    for h in range(2):
                        P.op("vector", lambda e, h=h, vt=vt: e.bn_stats(out=stt2[:, h * 6:(h + 1) * 6], in_=vt[:, h * 512:(h + 1) * 512]),
                             reads=[V], writes=["stt2"])
                    P.op("vector", lambda e: e.bn_aggr(out=mvt2[:, 0:2], in_=stt2[:, 0:12]), reads=["stt2"], writes=["mv2"])
                    P.op("scalar", lambda e: e.activation(out=mvt2[:, 2:3], in_=mvt2[:, 1:2], func=AF.Sqrt, bias=epsc[:, 0:1], scale=1.0),
                         reads=["mv2", "epsc"], writes=["sd2"])
                    P.op("vector", lambda e: e.reciprocal(out=mvt2[:, 3:4], in_=mvt2[:, 2:3]), reads=["sd2"], writes=["rstd2"])
                    P.op("vector", lambda e: e.tensor_scalar(out=mvt2[:, 4:5], in0=mvt2[:, 0:1], scalar1=mvt2[:, 3:4], scalar2=-1.0,
                                                             op0=ALU.mult, op1=ALU.mult), reads=["mv2", "rstd2"], writes=["nmr2"])
                    P.op("scalar", lambda e, vt=vt: e.activation(out=vt, in_=vt, func=AF.Identity, bias=mvt2[:, 4:5], scale=mvt2[:, 3:4]),
                         reads=[V, "nmr2", "rstd2"], writes=[V])
                    P.op("vector", lambda e, vt=vt: e.tensor_tensor(out=vt, in0=vt, in1=lnvg_b, op=ALU.mult), reads=[V, "lnvg_b"], writes=[V])
                    P.op("vector", lambda e, vt=vt, tt=tt: e.tensor_tensor(out=vln16[tt], in0=vt, in1=lnvb_b, op=ALU.add),
                         reads=[V, "lnvb_b"], writes=["vln%d" % tt])
                for h in range(8):
                    pb = 6 + h % 2
                    for tt in range(4):
                        P.op("tensor", lambda e, h=h, tt=tt, pb=pb: e.matmul(bank(pb)[:, tt * 128:(tt + 1) * 128],
                                                                          lhsT=vln16[tt][:, h * 128:(h + 1) * 128], rhs=wssb[:, h, :],
                                                                          start=True, stop=True),
                             reads=["vln%d" % tt, "wssb"], writes=["B%d" % pb], inc=(tt == 3))
                    sv_ = svt[h % 2]
                    P.op("vector", lambda e, h=h, pb=pb, sv_=sv_: e.tensor_tensor(
                        out=sv_.rearrange("p (a t) -> p a t", t=128), in0=bank(pb).rearrange("p (a t) -> p a t", t=128),
                        in1=bs_b[:, h * 128:(h + 1) * 128].unsqueeze(1).to_broadcast([128, 4, 128]), op=ALU.add),
                        reads=["B%d" % pb, "bs_b"], writes=["svt%d" % (h % 2)])
                    P.op("vector", lambda e, h=h, sv_=sv_: e.tensor_tensor(out=gT16[:, h, :], in0=sv_, in1=uT[:, h, :], op=ALU.mult),
                         reads=["svt%d" % (h % 2), "uT"], writes=["gT16"])
                for tt in range(4):
                    for half in range(2):
                        for c in range(8):
                            P.op("tensor", lambda e, c=c, tt=tt, half=half: e.matmul(
                                bank(half), lhsT=gT16[:, c, tt * 128:(tt + 1) * 128], rhs=woutsb[:, c, half * 512:(half + 1) * 512],
                                start=(c == 0), stop=(c == 7)), reads=["gT16", "woutsb"], writes=["B%d" % half], inc=(c == 7))
                    ln_epilogue(xg3[:, tt, :], psum[:, 0:2, :].rearrange("p a n -> p (a n)"), ["B0", "B1"], bout_b,
                                ve, gate_b, lng_b, lnb_b, stt, mvt)
                group_store(xg3, t0)
            P.barrier()

        def emit_ab(l, src):
            sub = 2 * l
            j = l // 2
            A32.reset(); A16.reset()
            winsb = A16.alloc(8 * 1536).rearrange("p (k n) -> p k n", n=1536)
            woutsb = A16.alloc(8 * 1024).rearrange("p (k n) -> p k n", n=1024)
            pwsb = A16.alloc(4 * 128).rearrange("p (g d) -> p g d", d=128)
            bdsb = A16.alloc(2 * 4 * 128).rearrange("p (w c j) -> p w c j", w=2, c=4)
            hT16 = A16.alloc(8 * 512).rearrange("p (k n) -> p k n", n=512)
            pooled16 = A16.alloc(4 * 512).rearrange("p (g n) -> p g n", n=512)
            xc16 = A16.alloc(512)
            ymixT = A16.alloc(8 * 512).rearrange("p (k n) -> p k n", n=512)
            stgR = A32.alloc(6400)
            for i in range(3):
                stg_sl[i] = stgR[:, i * 2048:(i + 1) * 2048]
            xg3 = stgR[:, 0:4096].rearrange("p (t d) -> p t d", d=1024)
            ve = stgR[:, 4096:5120]
            sA = stgR[:, 5120:5120 + 528]; sB = stgR[:, 5120 + 528:5120 + 1056]
            tmp16 = stgR[:, 5120 + 1056:5120 + 1072]
            bout_b = A32.alloc(1024); gate_b = A32.alloc(1024); lng_b = A32.alloc(1024); lnb_b = A32.alloc(1024)
            xpb = A32.alloc(4 * 528).rearrange("p (g n) -> p g n", n=528)
            xrb = A32.alloc(4 * 516).rearrange("p (g n) -> p g n", n=516)
            xc32 = A32.alloc(512); rr_ = A32.alloc(512); ig = A32.alloc(512); aa = A32.alloc(512); mm_ = A32.alloc(512)
            hs = A32.alloc(512); gg = A32.alloc(512)
            avec = A32.alloc(40); spv = A32.alloc(4); m8 = A32.alloc(4); m16 = A32.alloc(4); hst = A32.alloc(4)
            invc = A32.alloc(64).rearrange("p (g n) -> p g n", n=16); iotf = A32.alloc(16)
            shT = A32.alloc(8); scT = A32.alloc(8); stt = A32.alloc(12); mvt = A32.alloc(8)

            pieces = []
            for kc in range(8):
                pieces.append((ab_w_in[j, kc * 128:(kc + 1) * 128, :], winsb[:, kc, :], (lambda a: a[:, 0:1536]), "winsb", "scalar"))
            for q in range(4):
                pieces.append((ab_w_out[j, q * 256:(q + 1) * 256, :].rearrange("(k p) n -> p k n", p=128), woutsb[:, 2 * q:2 * q + 2, :],
                               (lambda a: a.rearrange("p (k n) -> p k n", n=1024)), "woutsb", "vector"))
            pieces.append((pool_w[j].rearrange("g c d -> c g d"), pwsb, (lambda a: a[:, 0:512].rearrange("p (g d) -> p g d", d=128)), "pwsb", "vector"))
            pieces.append((lru_bd[j].rearrange("w c i j -> i w c j"), bdsb,
                           (lambda a: a[:, 0:1024].rearrange("p (w c j) -> p w c j", w=2, c=4)), "bdsb", "vector"))
            load_weights_bf16(pieces)
            P.dma("sync", "avec", lambda e: e.dma_start(out=avec, in_=ab_vec[j]), writes=["avec"])
            pscale = avec[:, 0:4]; cb_ = avec[:, 4:8]; ba_ = avec[:, 8:12]; bx_ = avec[:, 12:16]; lam = avec[:, 16:20]
            P.op("scalar", lambda e: e.activation(out=spv, in_=lam, func=AF.Exp, scale=-1.0), reads=["avec"], writes=["spv"])
            P.op("scalar", lambda e: e.activation(out=spv, in_=spv, func=AF.Ln, bias=onec[:, 0:1], scale=1.0), reads=["spv", "onec"], writes=["spv"])
            P.op("vector", lambda e: e.tensor_scalar(out=m8, in0=spv, scalar1=-8.0, scalar2=None, op0=ALU.mult), reads=["spv"], writes=["m8"])
            P.op("vector", lambda e: e.tensor_scalar(out=m16, in0=spv, scalar1=-16.0, scalar2=None, op0=ALU.mult), reads=["spv"], writes=["m16"])
            P.op("gpsimd", lambda e: e.iota(iotf, pattern=[[1, 16]], base=1, channel_multiplier=0, allow_small_or_imprecise_dtypes=True),
                 writes=["iotf"])
            for g_ in range(4):
                P.op("vector", lambda e, g_=g_: e.tensor_scalar(out=invc[:, g_, :], in0=iotf, scalar1=float(2 ** (g_ + 1)), scalar2=None, op0=ALU.min),
                     reads=["iotf"], writes=["invc"])
            P.op("vector", lambda e: e.reciprocal(out=invc, in_=invc), reads=["invc"], writes=["invc"])
            bload(bout_b, ab_b_out[j], "bout_b"); bload(lng_b, ln_g[l, 0], "lng_b"); bload(lnb_b, ln_b[l, 0], "lnb_b")
            P.barrier()

            for g in range(T // 512):
                t0 = g * 512
                b = t0 // S
                seq_start = (t0 % S == 0)
                if seq_start:
                    P.new_segment(None)
                    bload(gate_b, m_dram[sub, b, 2 * D:3 * D], "gate_b")
                    load_mod_vectors(sub, b, shT, scT, None, "")
                    P.op("vector", lambda e: e.memset(xpb[:, :, 0:16], 0.0), writes=["xpb0", "xpb1", "xpb2", "xpb3"])
                    P.op("vector", lambda e: e.memset(xrb[:, :, 0:4], 0.0), writes=["xrb0", "xrb1", "xrb2", "xrb3"])
                    P.op("vector", lambda e: e.memset(hst, 0.0), writes=["hst"])
                mixer_prologue(src, t0, hT16, xg3, shT, scT)

                def zchunk(c, pb):
                    for k in range(8):
                        P.op("tensor", lambda e, k=k: e.matmul(bank(pb), lhsT=winsb[:, k, c * 128:(c + 1) * 128], rhs=hT16[:, k, :],
                                                             start=(k == 0), stop=(k == 7)),
                             reads=["winsb", "hT16"], writes=["B%d" % pb], inc=(k == 7))

                for g_ in range(4):
                    pb = 2 + g_ % 2
                    X = "xpb%d" % g_
                    zchunk(g_, pb)
                    P.op("scalar", lambda e, g_=g_, pb=pb: e.activation(out=xpb[:, g_, 16:528], in_=bank(pb), func=AF.Copy),
                         reads=["B%d" % pb], writes=[X])
                    cur, cres = xpb[:, g_, :], X
                    bufs = [(sA, "sA"), (sB, "sB")]
                    for lev in range(g_ + 1):
                        sh = 2 ** lev
                        lo = 2 * sh - 1
                        nx, nres = bufs[lev % 2]
                        P.op("vector", lambda e, cur=cur, nx=nx, sh=sh, lo=lo: e.tensor_tensor(
                            out=nx[:, lo:528], in0=cur[:, lo:528], in1=cur[:, lo - sh:528 - sh], op=ALU.add),
                            reads=[cres], writes=[nres])
                        cur, cres = nx, nres
                    w_ = float(2 ** (g_ + 1))
                    P.op("vector", lambda e, cur=cur, g_=g_, w_=w_: e.scalar_tensor_tensor(
                        out=pooled16[:, g_, :], in0=cur[:, 16:528], scalar=1.0 / w_, in1=xpb[:, g_, 16:528], op0=ALU.mult, op1=ALU.subtract),
                        reads=[cres, X], writes=["pooled%d" % g_])
                    if seq_start:
                        P.op("vector", lambda e, cur=cur, g_=g_: e.tensor_tensor(out=tmp16, in0=cur[:, 16:32], in1=invc[:, g_, :], op=ALU.mult),
                             reads=[cres, "invc"], writes=["tmp16"])
                        P.op("vector", lambda e, g_=g_: e.tensor_tensor(out=pooled16[:, g_, 0:16], in0=tmp16, in1=xpb[:, g_, 16:32], op=ALU.subtract),
                             reads=["tmp16", X], writes=["pooled%d" % g_])
                    P.op("vector", lambda e, g_=g_: e.tensor_copy(out=xpb[:, g_, 0:16], in_=xpb[:, g_, 512:528]), reads=[X], writes=[X])
                    pb2 = 4 + g_ % 2
                    P.op("tensor", lambda e, g_=g_, pb2=pb2: e.matmul(bank(pb2), lhsT=pwsb[:, g_, :], rhs=pooled16[:, g_, :], start=True, stop=True),
                         reads=["pwsb", "pooled%d" % g_], writes=["B%d" % pb2])
                    P.op("scalar", lambda e, g_=g_, pb2=pb2: e.activation(out=ymixT[:, g_, :], in_=bank(pb2), func=AF.Identity,
                                                                        bias=zeroc[:, 0:1], scale=pscale[:, g_:g_ + 1]),
                         reads=["B%d" % pb2, "avec", "zeroc"], writes=["ymixT"])
                for c in range(4):
                    pb = 2 + c % 2
                    X = "xrb%d" % c
                    zchunk(4 + c, pb)
                    P.op("scalar", lambda e, c=c, pb=pb: e.activation(out=xrb[:, c, 4:516], in_=bank(pb), func=AF.Copy),
                         reads=["B%d" % pb], writes=[X])
                    cw = lambda tap, c=c: avec[:, 20 + c * 4 + tap:21 + c * 4 + tap]
                    P.op("vector", lambda e, c=c, cw=cw: e.tensor_scalar(out=xc32, in0=xrb[:, c, 4:516], scalar1=cw(3), scalar2=cb_[:, c:c + 1],
                                                                       op0=ALU.mult, op1=ALU.add), reads=[X, "avec"], writes=["xc32"])
                    for d_ in (1, 2, 3):
                        P.op("vector", lambda e, c=c, cw=cw, d_=d_: e.scalar_tensor_tensor(
                            out=xc32, in0=xrb[:, c, 4 - d_:516 - d_], scalar=cw(3 - d_), in1=xc32, op0=ALU.mult, op1=ALU.add),
                            reads=[X, "avec", "xc32"], writes=["xc32"])
                    P.op("vector", lambda e, c=c: e.tensor_copy(out=xrb[:, c, 0:4], in_=xrb[:, c, 512:516]), reads=[X], writes=[X])
                    P.op("scalar", lambda e: e.activation(out=xc16, in_=xc32, func=AF.Copy), reads=["xc32"], writes=["xc16"])
                    for w_i, pbw in ((0, 4), (1, 5)):
                        P.op("tensor", lambda e, c=c, w_i=w_i, pbw=pbw: e.matmul(bank(pbw), lhsT=bdsb[:, w_i, c, :], rhs=xc16, start=True, stop=True),
                             reads=["bdsb", "xc16"], writes=["B%d" % pbw])
                    P.op("scalar", lambda e, c=c: e.activation(out=rr_, in_=bank(4), func=AF.Sigmoid, bias=ba_[:, c:c + 1], scale=1.0),
                         reads=["B4", "avec"], writes=["rr_"])
                    P.op("scalar", lambda e, c=c: e.activation(out=ig, in_=bank(5), func=AF.Sigmoid, bias=bx_[:, c:c + 1], scale=1.0),
                         reads=["B5", "avec"], writes=["ig"])
                    P.op("scalar", lambda e, c=c: e.activation(out=aa, in_=rr_, func=AF.Exp, scale=m8[:, c:c + 1]), reads=["rr_", "m8"], writes=["aa"])
                    P.op("scalar", lambda e, c=c: e.activation(out=mm_, in_=rr_, func=AF.Exp, scale=m16[:, c:c + 1]), reads=["rr_", "m16"], writes=["mm_"])
                    P.op("scalar", lambda e: e.activation(out=mm_, in_=mm_, func=AF.Sqrt, bias=onec[:, 0:1], scale=-1.0),
                         reads=["mm_", "onec"], writes=["mm_"])
                    P.op("vector", lambda e: e.tensor_tensor(out=mm_, in0=mm_, in1=ig, op=ALU.mult), reads=["mm_", "ig"], writes=["mm_"])
                    P.op("vector", lambda e: e.tensor_tensor(out=mm_, in0=mm_, in1=xc32, op=ALU.mult), reads=["mm_", "xc32"], writes=["mm_"])
                    P.op("vector", lambda e, c=c: e.tensor_tensor_scan(out=hs, data0=aa, data1=mm_, initial=hst[:, c:c + 1],
                                                                     op0=ALU.mult, op1=ALU.add), reads=["aa", "mm_", "hst"], writes=["hs"])
                    P.op("vector", lambda e, c=c: e.tensor_copy(out=hst[:, c:c + 1], in_=hs[:, 511:512]), reads=["hs"], writes=["hst"])
                    pbg = 6 + c % 2
                    zchunk(8 + c, pbg)
                    P.op("scalar", lambda e, pbg=pbg: e.activation(out=gg, in_=bank(pbg), func=AF.Gelu_apprx_tanh), reads=["B%d" % pbg], writes=["gg"])
                    P.op("vector", lambda e, c=c: e.tensor_tensor(out=ymixT[:, 4 + c, :], in0=hs, in1=gg, op=ALU.mult),
                         reads=["hs", "gg"], writes=["ymixT"])
                for tt in range(4):
                    for half in range(2):
                        for c in range(8):
                            P.op("tensor", lambda e, c=c, tt=tt, half=half: e.matmul(
                                bank(half), lhsT=ymixT[:, c, tt * 128:(tt + 1) * 128], rhs=woutsb[:, c, half * 512:(half + 1) * 512],
                                start=(c == 0), stop=(c == 7)), reads=["ymixT", "woutsb"], writes=["B%d" % half], inc=(c == 7))
                    ln_epilogue(xg3[:, tt, :], psum[:, 0:2, :].rearrange("p a n -> p (a n)"), ["B0", "B1"], bout_b,
                                ve, gate_b, lng_b, lnb_b, stt, mvt)
                group_store(xg3, t0)
            P.barrier()

        try:
            emit_modulation()
            dbg_stop("mod")
            cur = x_in
            for sub in sublayers:
                l, s = sub // 2, sub % 2
                if s == 1:
                    emit_moe(l, cur)
                elif l % 2 == 0:
                    emit_ab(l, cur)
                else:
                    emit_sgu(l, cur)
                cur = y
        except StopBuild:
            pass
        P.barrier()
        P.build()
    return nc


def layout_inputs(inp, b0, nb, LW=4):
    f = lambda a: np.ascontiguousarray(np.asarray(a, dtype=np.float32))
    LE = (LW + 1) // 2
    LO = max(LW // 2, 1)
    m = {}
    S = inp["x"].shape[1]
    m["x"] = f(inp["x"][b0:b0 + nb]).reshape(nb * S, D)
    m["cT"] = f(np.asarray(inp["c"])[b0:b0 + nb].reshape(nb, 8, 128).transpose(2, 1, 0))
    for k in ("mod_w", "mod_b", "ln_g", "ln_b", "router_w", "router_b", "moe_w1", "moe_w2", "moe_b2"):
        m[k] = f(inp[k][:LW])
    m["moe_b1T"] = f(np.asarray(inp["moe_b1"])[:LW].reshape(LW, NE, 8, 128, 2).transpose(0, 3, 2, 4, 1).reshape(LW, 128, 512))
    for k in ("ab_w_in", "pool_w", "ab_w_out", "ab_b_out"):
        m[k] = f(inp[k][:LE])
    pv = lambda a: np.asarray(a)[:LE].reshape(LE, 4, 128).transpose(0, 2, 1)
    cw = np.asarray(inp["conv_w"])[:LE].reshape(LE, 4, 4, 128).transpose(0, 3, 2, 1)
    m["ab_vec"] = f(np.concatenate([pv(inp["pool_scale"]), pv(inp["conv_b"]), pv(inp["lru_b_a"]), pv(inp["lru_b_x"]),
                                    pv(inp["lru_lambda"]), cw.reshape(LE, 128, 16), np.zeros((LE, 128, 4), np.float32)], axis=2))
    bd = np.zeros((LE, 2, 4, 128, 128), np.float32)
    for wi, k in enumerate(("lru_w_a", "lru_w_x")):
        w = np.asarray(inp[k])[:LE]
        for c in range(4):
            bd[:, wi, c, 0:64, 0:64] = w[:, 2 * c]
            bd[:, wi, c, 64:128, 64:128] = w[:, 2 * c + 1]
    m["lru_bd"] = bd
    for k in ("sgu_w_in", "sgu_ln_g", "sgu_ln_b", "sgu_w_out", "sgu_b_out"):
        m[k] = f(inp[k][:LO])
    bi = np.asarray(inp["sgu_b_in"])[:LO]
    m["sgu_buT"] = f(bi[:, :D].reshape(LO, 8, 128).transpose(0, 2, 1))
    m["sgu_bv"] = f(bi[:, D:])
    m["sgu_wsT"] = f(np.asarray(inp["sgu_w_s"])[:LO].transpose(0, 1, 3, 2))
    m["sgu_b_s"] = f(np.asarray(inp["sgu_b_s"])[:LO].reshape(LO, 8 * 128))
    return m


_NC_CACHE = {}


def kernel(**inputs):
    n = 8
    B = inputs["x"].shape[0]
    S = inputs["x"].shape[1]
    nb = B // n
    key = (nb, S)
    if key not in _NC_CACHE:
        _NC_CACHE[key] = build_program(nb=nb, S=S)
    nc = _NC_CACHE[key]
    shared = layout_inputs(inputs, 0, nb)
    in_maps = []
    for i in range(n):
        m = dict(shared)
        m["x"] = np.ascontiguousarray(np.asarray(inputs["x"], dtype=np.float32)[i * nb:(i + 1) * nb]).reshape(nb * S, D)
        m["cT"] = np.ascontiguousarray(np.asarray(inputs["c"], dtype=np.float32)[i * nb:(i + 1) * nb].reshape(nb, 8, 128).transpose(2, 1, 0))
        in_maps.append(m)
    res = run_bass_kernel_spmd(nc, in_maps, core_ids=list(range(n)))
    out = np.stack([np.asarray(r["y"]).reshape(nb, S, D) for r in res.results], axis=0)
    return out.reshape(B, S, D).astype(np.float32)
```
